# Optimizing a Trainium2 kernel written in Bass

```python
import math
import jax
import jax.numpy as jnp
from jax import lax
import numpy as np

D_MODEL = 2048
BATCH = 4
SEQ = 4096
DEPTH = 4

N_EVEN = (DEPTH + 1) // 2
N_ODD = DEPTH // 2
Q_BLOCK = 128

HEAD_DIM = 128
SB_HEADS = 8
FOX_HEADS = 8
SB_WIDTH = SB_HEADS * HEAD_DIM
FOX_WIDTH = FOX_HEADS * HEAD_DIM
EVEN_MIX = SB_WIDTH + FOX_WIDTH
EVEN_IN = 3 * SB_WIDTH + 3 * FOX_WIDTH + FOX_HEADS
EVEN_SPLITS = (SB_WIDTH, 2 * SB_WIDTH, 3 * SB_WIDTH,
               3 * SB_WIDTH + FOX_WIDTH, 3 * SB_WIDTH + 2 * FOX_WIDTH,
               3 * SB_WIDTH + 3 * FOX_WIDTH)

MLA_HEADS = 16
Q_LORA = 512
KV_LORA = 512
NOPE_DIM = 128
ROPE_DIM = 64
V_DIM = 128
ROPE_THETA = 10000.0
MLA_DOWN = Q_LORA + KV_LORA + ROPE_DIM

N_EXPERTS = 32
TOP_K = 4
D_EXPERT = 768
SWIGLU_LIMIT = 7.0
SWIGLU_ALPHA = 1.702
EXPERT_BLOCK = 128

DEEPNORM_ALPHA = (2 * DEPTH) ** 0.25
DEEPNORM_BETA = (8 * DEPTH) ** -0.25
LN_EPS = 1e-5
RMS_EPS = 1e-6

kernel_name = "hybrid_stickbreak_fox_mla_moe_deepnorm"


def _layer_norm(x, g, b):
    xf = x.astype(jnp.float32)
    mu = jnp.mean(xf, axis=-1, keepdims=True)
    var = jnp.mean(jnp.square(xf - mu), axis=-1, keepdims=True)
    y = (xf - mu) * lax.rsqrt(var + LN_EPS) * g.astype(jnp.float32) + b.astype(jnp.float32)
    return y.astype(x.dtype)


def _rms_norm(x, g):
    xf = x.astype(jnp.float32)
    y = xf * lax.rsqrt(jnp.mean(xf * xf, axis=-1, keepdims=True) + RMS_EPS) * g.astype(jnp.float32)
    return y.astype(x.dtype)


def _query_blocks(t):
    b, s = t.shape[0], t.shape[1]
    t = t.reshape((b, s // Q_BLOCK, Q_BLOCK) + t.shape[2:])
    return jnp.moveaxis(t, 1, 0)


def _merge_blocks(o):
    o = jnp.moveaxis(o, 0, 1)
    return o.reshape((o.shape[0], o.shape[1] * o.shape[2]) + o.shape[3:])


def _stick_breaking_attention(q, k, v):
    seq = q.shape[1]
    scale = HEAD_DIM ** -0.5
    kpos = jnp.arange(seq)
    starts = jnp.arange(seq // Q_BLOCK) * Q_BLOCK

    def block(args):
        qb, start = args
        qpos = start + jnp.arange(Q_BLOCK)
        z = jnp.einsum('bqhd,bkhd->bhqk', qb, k,
                       preferred_element_type=jnp.float32) * scale
        strict = kpos[None, :] < qpos[:, None]
        log_fail = jnp.where(strict, -jax.nn.softplus(z), 0.0)
        later = lax.cumsum(log_fail, axis=3, reverse=True) - log_fail
        w = jnp.where(strict, jnp.exp(jax.nn.log_sigmoid(z) + later), 0.0)
        return jnp.einsum('bhqk,bkhd->bqhd', w.astype(v.dtype), v)

    return _merge_blocks(lax.map(block, (_query_blocks(q), starts)))


def _forgetting_attention(q, k, v, log_f):
    seq = q.shape[1]
    scale = HEAD_DIM ** -0.5
    kpos = jnp.arange(seq)
    starts = jnp.arange(seq // Q_BLOCK) * Q_BLOCK
    cum = lax.cumsum(log_f, axis=1)
    cum_k = jnp.transpose(cum, (0, 2, 1))

    def block(args):
        qb, cum_q, start = args
        qpos = start + jnp.arange(Q_BLOCK)
        z = jnp.einsum('bqhd,bkhd->bhqk', qb, k,
                       preferred_element_type=jnp.float32) * scale
        decay = jnp.transpose(cum_q, (0, 2, 1))[:, :, :, None] - cum_k[:, :, None, :]
        causal = kpos[None, :] <= qpos[:, None]
        p = jax.nn.softmax(jnp.where(causal, z + decay, -jnp.inf), axis=-1)
        return jnp.einsum('bhqk,bkhd->bqhd', p.astype(v.dtype), v)

    return _merge_blocks(lax.map(block, (_query_blocks(q), _query_blocks(cum), starts)))


def _sb_fox_mixer(x, w_in, b_f, w_o):
    b, s, _ = x.shape
    h = x @ w_in
    q_sb, k_sb, v_sb, q_fx, k_fx, v_fx, f_logit = jnp.split(h, list(EVEN_SPLITS), axis=-1)
    sb = lambda t: t.reshape(b, s, SB_HEADS, HEAD_DIM)
    fx = lambda t: t.reshape(b, s, FOX_HEADS, HEAD_DIM)
    o_sb = _stick_breaking_attention(sb(q_sb), sb(k_sb), sb(v_sb))
    log_f = jax.nn.log_sigmoid(f_logit.astype(jnp.float32) + b_f.astype(jnp.float32))
    o_fx = _forgetting_attention(fx(q_fx), fx(k_fx), fx(v_fx), log_f)
    o = jnp.concatenate([o_sb.reshape(b, s, SB_WIDTH), o_fx.reshape(b, s, FOX_WIDTH)], axis=-1)
    return (o @ w_o).astype(x.dtype)


def _rope(t, cos, sin):
    t1, t2 = jnp.split(t, 2, axis=-1)
    return jnp.concatenate([t1 * cos - t2 * sin, t1 * sin + t2 * cos], axis=-1)


def _mla_mixer(x, positions, w_down, q_norm, kv_norm, w_uq, w_ukv, w_o):
    b, s, _ = x.shape
    down = x @ w_down
    c_q, c_kv, k_pe = jnp.split(down, [Q_LORA, Q_LORA + KV_LORA], axis=-1)
    q = (_rms_norm(c_q, q_norm) @ w_uq).reshape(b, s, MLA_HEADS, NOPE_DIM + ROPE_DIM)
    q_nope, q_pe = jnp.split(q, [NOPE_DIM], axis=-1)
    kv = (_rms_norm(c_kv, kv_norm) @ w_ukv).reshape(b, s, MLA_HEADS, NOPE_DIM + V_DIM)
    k_nope, v = jnp.split(kv, [NOPE_DIM], axis=-1)

    inv_freq = ROPE_THETA ** (-jnp.arange(0, ROPE_DIM, 2, dtype=jnp.float32) / ROPE_DIM)
    ang = positions.astype(jnp.float32)[..., None] * inv_freq
    cos, sin = jnp.cos(ang), jnp.sin(ang)
    q_pe = _rope(q_pe, cos[:, :, None, :], sin[:, :, None, :]).astype(x.dtype)
    k_pe = _rope(k_pe, cos, sin).astype(x.dtype)

    scale = (NOPE_DIM + ROPE_DIM) ** -0.5
    kpos = jnp.arange(s)
    starts = jnp.arange(s // Q_BLOCK) * Q_BLOCK

    def block(args):
        qn, qp, start = args
        qpos = start + jnp.arange(Q_BLOCK)
        z = (jnp.einsum('bqhd,bkhd->bhqk', qn, k_nope, preferred_element_type=jnp.float32)
             + jnp.einsum('bqhr,bkr->bhqk', qp, k_pe, preferred_element_type=jnp.float32)) * scale
        causal = kpos[None, :] <= qpos[:, None]
        p = jax.nn.softmax(jnp.where(causal, z, -jnp.inf), axis=-1)
        return jnp.einsum('bhqk,bkhd->bqhd', p.astype(v.dtype), v)

    o = _merge_blocks(lax.map(block, (_query_blocks(q_nope), _query_blocks(q_pe), starts)))
    return (o.reshape(b, s, MLA_HEADS * V_DIM) @ w_o).astype(x.dtype)


def _moe(x, router_w, router_b, w_gu, b_gu, w_dn, b_dn):
    b, s, d = x.shape
    xt = x.reshape(-1, d)
    n = xt.shape[0]
    logits = jnp.matmul(xt, router_w, preferred_element_type=jnp.float32) + router_b.astype(jnp.float32)
    top_logit, top_e = lax.top_k(logits, TOP_K)
    gate = jax.nn.softmax(top_logit, axis=-1)

    n_assign = n * TOP_K
    flat_e = top_e.reshape(-1)
    order = jnp.argsort(flat_e)
    sorted_e = flat_e[order]
    sorted_tok = (order // TOP_K).astype(jnp.int32)
    sorted_gate = gate.reshape(-1)[order]
    counts = jnp.bincount(flat_e, length=N_EXPERTS)
    padded = (counts + EXPERT_BLOCK - 1) // EXPERT_BLOCK * EXPERT_BLOCK
    start = jnp.cumsum(counts) - counts
    pend = jnp.cumsum(padded)
    pstart = pend - padded
    dest = pstart[sorted_e] + jnp.arange(n_assign) - start[sorted_e]
    n_rows = -(-(n_assign + N_EXPERTS * (EXPERT_BLOCK - 1)) // EXPERT_BLOCK) * EXPERT_BLOCK
    n_blk = n_rows // EXPERT_BLOCK
    row_tok = jnp.full((n_rows,), n, jnp.int32).at[dest].set(sorted_tok)
    row_gate = jnp.zeros((n_rows,), jnp.float32).at[dest].set(sorted_gate)
    blk_e = jnp.minimum(jnp.searchsorted(pend, jnp.arange(n_blk) * EXPERT_BLOCK, side='right'),
                        N_EXPERTS - 1)
    x_pad = jnp.concatenate([xt, jnp.zeros((1, d), xt.dtype)], axis=0)

    def expert_block(args):
        tok, e = args
        hb = x_pad[tok] @ w_gu[e] + b_gu[e]
        g, u = jnp.split(hb, 2, axis=-1)
        g = jnp.minimum(g, SWIGLU_LIMIT)
        u = jnp.clip(u, -SWIGLU_LIMIT, SWIGLU_LIMIT)
        act = (u + 1.0) * (g * jax.nn.sigmoid(SWIGLU_ALPHA * g))
        return act @ w_dn[e] + b_dn[e]

    y = lax.map(expert_block, (row_tok.reshape(n_blk, EXPERT_BLOCK), blk_e))
    y = y.reshape(n_rows, d).astype(jnp.float32) * row_gate[:, None]
    out = jax.ops.segment_sum(y, row_tok, num_segments=n + 1)[:n]
    return out.astype(x.dtype).reshape(b, s, d)


def setup_inputs(seed: int = 0) -> dict:
    key = jax.random.key(seed)
    ks = jax.random.split(key, 20)
    nrm = lambda k, shape, scale: jax.random.normal(k, shape, jnp.float32) * scale
    x = nrm(ks[0], (BATCH, SEQ, D_MODEL), 1.0)
    positions = jnp.tile(jnp.arange(SEQ, dtype=jnp.int32)[None, :], (BATCH, 1))
    ln_mix_g = 1.0 + nrm(ks[1], (DEPTH, D_MODEL), 0.02)
    ln_mix_b = nrm(ks[2], (DEPTH, D_MODEL), 0.02)
    ln_ffn_g = 1.0 + nrm(ks[3], (DEPTH, D_MODEL), 0.02)
    ln_ffn_b = nrm(ks[4], (DEPTH, D_MODEL), 0.02)
    even_w_in = nrm(ks[5], (N_EVEN, D_MODEL, EVEN_IN), D_MODEL ** -0.5)
    fox_b_f = 2.0 + nrm(ks[6], (N_EVEN, FOX_HEADS), 0.1)
    even_w_o = nrm(ks[7], (N_EVEN, EVEN_MIX, D_MODEL), EVEN_MIX ** -0.5 * DEEPNORM_BETA)
    mla_w_down = nrm(ks[8], (N_ODD, D_MODEL, MLA_DOWN), D_MODEL ** -0.5)
    mla_q_norm = 1.0 + nrm(ks[9], (N_ODD, Q_LORA), 0.02)
    mla_kv_norm = 1.0 + nrm(ks[10], (N_ODD, KV_LORA), 0.02)
    mla_w_uq = nrm(ks[11], (N_ODD, Q_LORA, MLA_HEADS * (NOPE_DIM + ROPE_DIM)), Q_LORA ** -0.5)
    mla_w_ukv = nrm(ks[12], (N_ODD, KV_LORA, MLA_HEADS * (NOPE_DIM + V_DIM)), KV_LORA ** -0.5)
    mla_w_o = nrm(ks[13], (N_ODD, MLA_HEADS * V_DIM, D_MODEL),
                  (MLA_HEADS * V_DIM) ** -0.5 * DEEPNORM_BETA)
    router_w = nrm(ks[14], (DEPTH, D_MODEL, N_EXPERTS), D_MODEL ** -0.5)
    router_b = nrm(ks[15], (DEPTH, N_EXPERTS), 0.01)
    expert_w_gate_up = nrm(ks[16], (DEPTH, N_EXPERTS, D_MODEL, 2 * D_EXPERT), D_MODEL ** -0.5)
    expert_b_gate_up = nrm(ks[17], (DEPTH, N_EXPERTS, 2 * D_EXPERT), 0.01)
    expert_w_down = nrm(ks[18], (DEPTH, N_EXPERTS, D_EXPERT, D_MODEL), D_EXPERT ** -0.5 * DEEPNORM_BETA)
    expert_b_down = nrm(ks[19], (DEPTH, N_EXPERTS, D_MODEL), 0.01)
    return {"x": x, "positions": positions,
            "ln_mix_g": ln_mix_g, "ln_mix_b": ln_mix_b, "ln_ffn_g": ln_ffn_g, "ln_ffn_b": ln_ffn_b,
            "even_w_in": even_w_in, "fox_b_f": fox_b_f, "even_w_o": even_w_o,
            "mla_w_down": mla_w_down, "mla_q_norm": mla_q_norm, "mla_kv_norm": mla_kv_norm,
            "mla_w_uq": mla_w_uq, "mla_w_ukv": mla_w_ukv, "mla_w_o": mla_w_o,
            "router_w": router_w, "router_b": router_b,
            "expert_w_gate_up": expert_w_gate_up, "expert_b_gate_up": expert_b_gate_up,
            "expert_w_down": expert_w_down, "expert_b_down": expert_b_down}


def reference(x, positions, ln_mix_g, ln_mix_b, ln_ffn_g, ln_ffn_b,
              even_w_in, fox_b_f, even_w_o,
              mla_w_down, mla_q_norm, mla_kv_norm, mla_w_uq, mla_w_ukv, mla_w_o,
              router_w, router_b, expert_w_gate_up, expert_b_gate_up,
              expert_w_down, expert_b_down):
    for layer in range(DEPTH):
        i = layer // 2
        if layer % 2 == 0:
            mix = _sb_fox_mixer(x, even_w_in[i], fox_b_f[i], even_w_o[i])
        else:
            mix = _mla_mixer(x, positions, mla_w_down[i], mla_q_norm[i], mla_kv_norm[i],
                             mla_w_uq[i], mla_w_ukv[i], mla_w_o[i])
        x = _layer_norm(DEEPNORM_ALPHA * x + mix, ln_mix_g[layer], ln_mix_b[layer])
        ffn = _moe(x, router_w[layer], router_b[layer], expert_w_gate_up[layer],
                   expert_b_gate_up[layer], expert_w_down[layer], expert_b_down[layer])
        x = _layer_norm(DEEPNORM_ALPHA * x + ffn, ln_ffn_g[layer], ln_ffn_b[layer])
    return x
```

```python
import os
import numpy as np
import ml_dtypes
from contextlib import ExitStack
import concourse.bass as bass
import concourse.mybir as mybir
from concourse.bass_utils import run_bass_kernel_spmd

F32 = mybir.dt.float32
BF16 = mybir.dt.bfloat16
I32 = mybir.dt.int32
ALU = mybir.AluOpType
AF = mybir.ActivationFunctionType
AX = mybir.AxisListType

ENGS = ['pe', 'act', 'dve', 'pool', 'sp']


class _Op:
    __slots__ = ('fn', 'deps', 'is_dma', 'stream', 'sidx', 'needed', 'ordv')


def _key(x):
    return getattr(x, 'key', x)


class Tile:
    def __init__(self, t, key):
        self.t = t
        self.key = key

    def __getitem__(self, idx):
        return self.t[idx]


class Sched:
    def __init__(self, nc, es, dma_slots=None, same_engine_sync=True):
        self.nc = nc
        self.ops = {e: [] for e in ENGS}
        self.streams = {}
        self.last_w = {}
        self.readers = {}
        self.seen = {e: {} for e in ENGS}
        self.pending = {e: set() for e in ENGS}
        self.same_engine_sync = same_engine_sync
        self.dma_slots = dma_slots or {'sp': 14, 'pool': 10}
        self.slot_rr = {q: 0 for q in self.dma_slots}
        self.sems = {}
        for e in ['pe', 'act', 'dve', 'pool']:
            self.streams[e] = []
            self.sems[e] = es.enter_context(nc.semaphore('s_' + e))
        for q, n in self.dma_slots.items():
            for i in range(n):
                st = ('dma', q, i)
                self.streams[st] = []
                self.sems[st] = es.enter_context(nc.semaphore('d_%s%d' % (q, i)))

    def _add_dep(self, eng, deps, ref):
        if ref is None:
            return
        st, idx = ref
        if st == eng and (eng == 'pe' or not self.same_engine_sync):
            return
        if self.seen[eng].get(st, -1) >= idx:
            return
        deps.add(ref)

    def _record(self, eng, fn, reads, writes, stream, is_dma):
        deps = set()
        for r in self.pending[eng]:
            self._add_dep(eng, deps, r)
        self.pending[eng] = set()
        for k in reads:
            self._add_dep(eng, deps, self.last_w.get(k))
        for k in writes:
            self._add_dep(eng, deps, self.last_w.get(k))
            for st, idx in self.readers.get(k, {}).items():
                self._add_dep(eng, deps, (st, idx))
        if is_dma:
            sl = self.streams[stream]
            if sl:
                self._add_dep(eng, deps, (stream, len(sl) - 1))
        best = {}
        for st, idx in deps:
            if best.get(st, -1) < idx:
                best[st] = idx
        for st, idx in best.items():
            self.seen[eng][st] = idx
            self.streams[st][idx].needed = True
        o = _Op()
        o.fn = fn
        o.deps = list(best.items())
        o.is_dma = is_dma
        o.stream = stream
        o.needed = is_dma
        sl = self.streams[stream]
        o.sidx = len(sl)
        sl.append(o)
        self.ops[eng].append(o)
        ref = (stream, o.sidx)
        for k in reads:
            self.readers.setdefault(k, {})[stream] = o.sidx
        for k in writes:
            self.last_w[k] = ref
            self.readers[k] = {}
        return ref

    def op(self, eng, fn, reads=(), writes=()):
        return self._record(eng, fn, [_key(r) for r in reads], [_key(w) for w in writes], eng, False)

    def dma(self, q, fn, reads=(), writes=()):
        n = self.dma_slots[q]
        i = self.slot_rr[q]
        self.slot_rr[q] = (i + 1) % n
        return self._record(q, fn, [_key(r) for r in reads], [_key(w) for w in writes], ('dma', q, i), True)

    def barrier(self):
        refs = set()
        for st, sl in self.streams.items():
            if sl:
                refs.add((st, len(sl) - 1))
        for e in ENGS:
            self.pending[e] |= refs

    def finish(self):
        self.barrier()
        for eng in ENGS:
            deps = set()
            for r in self.pending[eng]:
                self._add_dep(eng, deps, r)
            self.pending[eng] = set()
            best = {}
            for st, idx in deps:
                if best.get(st, -1) < idx:
                    best[st] = idx
            for st, idx in best.items():
                self.streams[st][idx].needed = True
            o = _Op()
            o.fn = None
            o.deps = list(best.items())
            o.is_dma = False
            o.stream = None
            o.needed = False
            o.sidx = -1
            self.ops[eng].append(o)

    def flush(self):
        self.barrier()
        for st, sl in self.streams.items():
            if sl:
                sl[-1].needed = True
        self._emit_block()

    def _emit_block(self):
        if not hasattr(self, 'emitted'):
            self.emitted = {e: 0 for e in ENGS}
            self.ord_cnt = {}
            self.ord_pos = {}
        for st, sl in self.streams.items():
            c = self.ord_cnt.get(st, 0)
            for o in sl[self.ord_pos.get(st, 0):]:
                if o.needed:
                    c += 1
                o.ordv = c
            self.ord_cnt[st] = c
            self.ord_pos[st] = len(sl)
        nc = self.nc
        self.block_idx = getattr(self, 'block_idx', 0) + 1
        with nc.Block() as block:
            def run(eng):
                def body(h):
                    ops = self.ops[eng]
                    for o in ops[self.emitted[eng]:]:
                        for st, idx in o.deps:
                            src = self.streams[st][idx]
                            h.wait_ge(self.sems[st], src.ordv * (16 if src.is_dma else 1))
                        if o.fn is None:
                            continue
                        ins = o.fn(h)
                        if o.needed:
                            ins.then_inc(self.sems[o.stream], 16 if o.is_dma else 1)
                    self.emitted[eng] = len(ops)
                return body
            block.tensor(run('pe'))
            block.scalar(run('act'))
            block.vector(run('dve'))
            block.gpsimd(run('pool'))
            block.sync(run('sp'))

    def emit(self):
        self._emit_block()


class Cfg:
    def __init__(self, S=4096, NL=4, C=640, E=32, stop=None):
        self.stop = stop
        self.S = S
        self.D = 2048
        self.KC = 16
        self.NL = NL
        self.E = E
        self.C = C
        self.F = 768
        self.NT = S // 128
        self.TCH = 512
        self.NTC = S // 512
        self.NSLOT = E * C
        self.alpha = (2 * 4) ** 0.25
        self.BIG = float(2 ** 20)


EVEN_IN = 6152


class Ctx:
    pass


_UID = [0]


def _sb(X, es, name, shape, dt):
    _UID[0] += 1
    name = '%s_%d' % (name, _UID[0])
    return Tile(es.enter_context(X.nc.sbuf_tensor(name, shape, dt)), name)


def build(cfg):
    nc = bass.Bass('TRN2', target_bir_lowering=False)
    S_, D, NL, E, C, F_ = cfg.S, cfg.D, cfg.NL, cfg.E, cfg.C, cfg.F
    NE = (NL + 1) // 2
    NO = NL // 2
    X = Ctx()
    X.nc = nc
    X.cfg = cfg
    dr = {}

    def din(name, shape, dt=F32):
        dr[name] = nc.dram_tensor(name, list(shape), dt, kind='ExternalInput').ap()

    din('x', [S_, D])
    din('positions', [1, S_], I32)
    for n in ['ln_mix_g', 'ln_mix_b', 'ln_ffn_g', 'ln_ffn_b']:
        din(n, [NL, D])
    din('even_w_in', [NE, D, EVEN_IN])
    din('fox_b_f', [NE, 8])
    din('even_w_o', [NE, 2048, D])
    din('mla_w_down', [max(NO, 1), D, 1088])
    din('mla_q_norm', [max(NO, 1), 512])
    din('mla_kv_norm', [max(NO, 1), 512])
    din('mla_w_uq', [max(NO, 1), 512, 3072])
    din('mla_w_ukv', [max(NO, 1), 512, 4096])
    din('mla_w_o', [max(NO, 1), 2048, D])
    din('router_w', [NL, D, E])
    din('router_b', [NL, E])
    din('expert_w_gate_up', [NL, E, D, 2 * F_])
    din('expert_b_gate_up', [NL, E, 2 * F_])
    din('expert_w_down', [NL, E, F_, D])
    din('expert_b_down', [NL, E, D])
    din('c_f32', [128, 512])
    din('c_bf', [128, 768], BF16)
    din('c_misc', [128, 64])
    dr['out'] = nc.dram_tensor('out', [S_, D], F32, kind='ExternalOutput').ap()

    def dscr(name, shape, dt):
        dr[name] = nc.dram_tensor(name, list(shape), dt).ap()

    dscr('xres', [S_, D], F32)
    dscr('xT', [16, 128, S_], BF16)
    dscr('qT', [16, 128, S_], BF16)
    dscr('kT', [16, 128, S_], BF16)
    dscr('qpT', [16, 64, S_], BF16)
    dscr('kpT', [64, S_], BF16)
    dscr('vtok', [S_, 2048], BF16)
    dscr('oT', [16, 128, S_], BF16)
    dscr('fxA', [8, 4, S_], BF16)
    dscr('fxB', [8, 4, S_], BF16)
    dscr('xslots', [cfg.NSLOT, D], BF16)
    dscr('yslots', [cfg.NSLOT, D], F32)
    X.dr = dr

    with ExitStack() as es:
        S = Sched(nc, es)
        X.S = S
        X.cf = _sb(X, es, 'cf', [128, 512], F32)
        X.cb = _sb(X, es, 'cb', [128, 768], BF16)
        X.cm = _sb(X, es, 'cm', [128, 64], F32)
        X.slots_all = _sb(X, es, 'slots_all', [128, cfg.NT, 4], I32)
        X.gates_all = _sb(X, es, 'gates_all', [128, cfg.NT, 4], F32)
        X.gbc = _sb(X, es, 'gbc', [128, D], F32)
        X.bbc = _sb(X, es, 'bbc', [128, D], F32)
        X.ps = [Tile(es.enter_context(nc.psum_tensor('ps%d' % i, [128, 512], F32)), 'ps%d' % i) for i in range(7)]
        X.pb = Tile(es.enter_context(nc.psum_tensor('psb', [128, 1024], BF16)), 'psb')
        X.ps_rrd = {}
        S.dma('sp', lambda e: e.dma_start(out=X.cf[:], in_=dr['c_f32'][:, :]), writes=[X.cf])
        S.dma('sp', lambda e: e.dma_start(out=X.cb[:], in_=dr['c_bf'][:, :]), writes=[X.cb])
        S.dma('sp', lambda e: e.dma_start(out=X.cm[:], in_=dr['c_misc'][:, :]), writes=[X.cm])
        X.ident_f = lambda: X.cf[:, 0:128]
        X.mask_lt_f = lambda: X.cf[:, 128:256]
        X.mask_le_f = lambda: X.cf[:, 256:384]
        X.ident_b = lambda: X.cb[:, 0:128]
        X.lt_b = lambda: X.cb[:, 128:256]
        X.ones_b = lambda: X.cb[:, 256:384]
        X.nge_b = lambda: X.cb[:, 384:512]
        X.zeros_b = lambda: X.cb[:, 512:640]
        X.le_b = lambda: X.cb[:, 640:768]

        phase_prologue(X)
        X.nph = 1
        for layer in range(NL if cfg.stop is None else 0):
            i = layer // 2
            if layer % 2 == 0:
                phase_even_proj(X, i)
                phase_attention(X, 'even')
                wo = dr['even_w_o'][i]
            else:
                phase_mla_proj(X, i)
                phase_attention(X, 'mla')
                wo = dr['mla_w_o'][i]
            phase_outproj_ln_route(X, layer, wo)
            phase_experts(X, layer)
            phase_combine_ln(X, layer, last=(layer == NL - 1))
        if cfg.stop is not None:
            debug_phases(X, cfg.stop)
        S.finish()
        S.emit()
    return nc


def debug_phases(X, stop):
    S, cfg, dr = X.S, X.cfg, X.dr
    seq = []
    for layer in range(cfg.NL):
        i = layer // 2
        if layer % 2 == 0:
            seq += [lambda i=i: phase_even_proj(X, i), lambda: phase_attention(X, 'even')]
            wo = dr['even_w_o'][i]
        else:
            seq += [lambda i=i: phase_mla_proj(X, i), lambda: phase_attention(X, 'mla')]
            wo = dr['mla_w_o'][i]
        seq += [lambda layer=layer, wo=wo: phase_outproj_ln_route(X, layer, wo), lambda layer=layer: phase_experts(X, layer),
                lambda layer=layer: phase_combine_ln(X, layer, last=False)]
    for f in (seq[:stop] if isinstance(stop, int) else [seq[j] for j in stop]):
        f()
    with ExitStack() as es:
        t = [_sb(X, es, 'dbg%d' % j, [128, cfg.D], F32) for j in range(2)]
        for tt in range(cfg.NT):
            S.dma('sp', lambda e, tt=tt: e.dma_start(out=t[tt % 2][:], in_=dr['xres'][tt * 128:(tt + 1) * 128, :]), reads=['xres'], writes=[t[tt % 2]])
            S.dma('sp', lambda e, tt=tt: e.dma_start(out=dr['out'][tt * 128:(tt + 1) * 128, :], in_=t[tt % 2][:]), reads=[t[tt % 2]], writes=['out'])
        S.flush()


def bc_reg(X, e):
    bi = X.S.block_idx
    if getattr(X, '_bc', (None, None))[0] != bi:
        X._bc = (bi, e.to_reg(X.cfg.NSLOT - 1))
    return X._bc[1]


def next_ps(X, lo=0, hi=7):
    k = (lo, hi)
    r = X.ps_rrd.get(k, 0)
    X.ps_rrd[k] = (r + 1) % (hi - lo)
    return X.ps[lo + r]


def load_bcast(X, dst, src_row_ap, n):
    X.S.dma('sp', lambda e: e.dma_start(out=dst[:, 0:n], in_=src_row_ap.partition_broadcast(128)), writes=[dst])


def layer_norm_tile(X, T, r, out, mult_eng='pool'):
    S = X.S
    D = X.cfg.D
    nch = D // 512
    st, mv, rstd, nmr = T['st'], T['mv'], T['rstd'], T['nmr']
    for c in range(nch):
        S.op('dve', lambda e, c=c: e.bn_stats(st[:, c, :], r[:, c * 512:(c + 1) * 512]), reads=[r], writes=[st])
    S.op('dve', lambda e: e.bn_aggr(mv[:], st[:]), reads=[st], writes=[mv])
    S.op('act', lambda e: e.activation(out=rstd[:], in_=mv[:, 1:2], func=AF.Sqrt, bias=X.cm[:, 3:4], scale=1.0), reads=[mv, X.cm], writes=[rstd])
    S.op('dve', lambda e: e.reciprocal(rstd[:], rstd[:]), reads=[rstd], writes=[rstd])
    S.op('dve', lambda e: e.tensor_scalar(nmr[:], mv[:, 0:1], rstd[:, 0:1], -1.0, ALU.mult, ALU.mult), reads=[mv, rstd], writes=[nmr])
    S.op('act', lambda e: e.activation(out=out[:], in_=r[:], func=AF.Identity, bias=nmr[:, 0:1], scale=rstd[:, 0:1]),
         reads=[r, nmr, rstd], writes=[out])
    S.op(mult_eng, lambda e: e.tensor_tensor(out[:], out[:], X.gbc[:], ALU.mult), reads=[out, X.gbc], writes=[out])
    S.op('dve', lambda e: e.tensor_tensor(out[:], out[:], X.bbc[:], ALU.add), reads=[out, X.bbc], writes=[out])


def make_xT_tile(X, T, xnew, t):
    S = X.S
    xb, xTt = T['xb'], T['xTt']
    S.op('act', lambda e: e.activation(out=xb[:], in_=xnew[:], func=AF.Copy), reads=[xnew], writes=[xb])
    for g in range(2):
        p = X.pb
        pv = lambda: X.pb[:]
        for j in range(8):
            kc = g * 8 + j
            S.op('pe', lambda e, kc=kc, j=j, pv=pv: e.transpose(pv()[:, j * 128:(j + 1) * 128], xb[:, kc * 128:(kc + 1) * 128], X.ident_b()),
                 reads=[xb, X.cb], writes=[p])
        eng = 'dve' if g == 0 else 'act'
        if eng == 'dve':
            S.op('dve', lambda e, g=g, pv=pv: e.tensor_copy(xTt[:, g * 8:(g + 1) * 8, :], pv().rearrange('p (a b) -> p a b', a=8)),
                 reads=[p], writes=[xTt])
        else:
            S.op('act', lambda e, g=g, pv=pv: e.activation(out=xTt[:, g * 8:(g + 1) * 8, :], in_=pv().rearrange('p (a b) -> p a b', a=8), func=AF.Copy),
                 reads=[p], writes=[xTt])
    dst = X.dr['xT'][:, :, t * 128:(t + 1) * 128].rearrange('k p s -> p k s')
    S.dma('sp', lambda e: e.dma_start(out=dst, in_=xTt[:]), reads=[xTt], writes=['xT'])


def phase_prologue(X):
    S, cfg, dr = X.S, X.cfg, X.dr
    with ExitStack() as es:
        T = {}
        for i in range(2):
            T['x%d' % i] = _sb(X, es, 'pr_x%d' % i, [128, cfg.D], F32)
            T['xb%d' % i] = _sb(X, es, 'pr_xb%d' % i, [128, cfg.D], BF16)
            T['xTt%d' % i] = _sb(X, es, 'pr_xT%d' % i, [128, 16, 128], BF16)
        for t in range(cfg.NT):
            xt = T['x%d' % (t % 2)]
            S.dma('sp', lambda e, t=t, xt=xt: e.dma_start(out=xt[:], in_=dr['x'][t * 128:(t + 1) * 128, :]), writes=[xt])
            S.dma('sp', lambda e, t=t, xt=xt: e.dma_start(out=dr['xres'][t * 128:(t + 1) * 128, :], in_=xt[:]), reads=[xt], writes=['xres'])
            if not os.environ.get('KDBG_NOXT'):
                make_xT_tile(X, {'xb': T['xb%d' % (t % 2)], 'xTt': T['xTt%d' % (t % 2)]}, xt, t)
        S.flush()


def load_w_slab(X, wt, w_ap, nk, ncols):
    src = w_ap.rearrange('(k p) n -> p k n', p=128)
    X.S.dma('pool', lambda e: e.dma_start(out=wt[:, 0:nk, 0:ncols], in_=src), writes=[wt])


def load_xin(X, xin, src_name, nk, tok0, ntok):
    src = X.dr[src_name][0:nk, :, tok0:tok0 + ntok].rearrange('k p s -> p k s')
    X.S.dma('sp', lambda e: e.dma_start(out=xin[:, 0:nk, 0:ntok], in_=src), reads=[src_name], writes=[xin])


def mm_fm(X, p, wt, nk, c0, M, xin, ntok, extra_reads=()):
    for kc in range(nk):
        X.S.op('pe', lambda e, kc=kc: e.matmul(p[0:M, 0:ntok], lhsT=wt[:, kc, c0:c0 + M], rhs=xin[:, kc, 0:ntok],
                                               start=(kc == 0), stop=(kc == nk - 1)),
               reads=[wt, xin] + list(extra_reads), writes=[p])


def evac(X, eng, out_ap_fn, in_ap_fn, reads, writes, scale=1.0):
    if eng == 'act':
        X.S.op('act', lambda e: e.activation(out=out_ap_fn(), in_=in_ap_fn(), func=AF.Copy, scale=scale), reads=reads, writes=writes)
    else:
        if scale == 1.0:
            X.S.op('dve', lambda e: e.tensor_copy(out_ap_fn(), in_ap_fn()), reads=reads, writes=writes)
        else:
            X.S.op('dve', lambda e: e.tensor_scalar(out_ap_fn(), in_ap_fn(), scale, None, ALU.mult), reads=reads, writes=writes)


def phase_even_proj(X, i):
    S, cfg, dr = X.S, X.cfg, X.dr
    S_ = cfg.S
    W = dr['even_w_in'][i]
    scale = 128 ** -0.5
    with ExitStack() as es:
        wt = [_sb(X, es, 'ep_w%d' % j, [128, 16, 512], BF16) for j in range(2)]
        xin = [_sb(X, es, 'ep_x%d' % j, [128, 16, 512], BF16) for j in range(2)]
        ot = [_sb(X, es, 'ep_o%d' % j, [128, 4, 512], BF16) for j in range(2)]
        wf = _sb(X, es, 'ep_wf', [128, 16, 8], BF16)
        fl = _sb(X, es, 'ep_fl', [8, S_], F32)
        ones8 = _sb(X, es, 'ep_ones', [8, S_], F32)
        bfc = _sb(X, es, 'ep_bf', [8, 1], F32)
        oi = 0
        xi = 0
        for sl in range(12):
            w = wt[sl % 2]
            load_w_slab(X, w, W[:, sl * 512:(sl + 1) * 512], 16, 512)
            grp = sl // 2
            for tc in range(cfg.NTC):
                xn = xin[xi % 2]
                xi += 1
                load_xin(X, xn, 'xT', 16, tc * 512, 512)
                o = ot[oi % 2]
                oi += 1
                if grp in (2, 5):
                    for tt in range(4):
                        p = next_ps(X)
                        for kc in range(16):
                            S.op('pe', lambda e, kc=kc, tt=tt, p=p, w=w, xn=xn: e.matmul(p[:, :], lhsT=xn[:, kc, tt * 128:(tt + 1) * 128], rhs=w[:, kc, :],
                                                                                         start=(kc == 0), stop=(kc == 15)),
                                 reads=[w, xn], writes=[p])
                        evac(X, 'act' if tt % 2 else 'dve', lambda o=o, tt=tt: o[:, tt, :], lambda p=p: p[:, :], [p], [o])
                    c0 = (0 if grp == 2 else 1024) + (sl % 2) * 512
                    dst = dr['vtok'][tc * 512:(tc + 1) * 512, c0:c0 + 512].rearrange('(a p) n -> p a n', p=128)
                    S.dma('sp', lambda e, dst=dst, o=o: e.dma_start(out=dst, in_=o[:]), reads=[o], writes=['vtok'])
                else:
                    isq = grp in (0, 3)
                    for sub in range(4):
                        p = next_ps(X)
                        mm_fm(X, p, w, 16, sub * 128, 128, xn, 512)
                        evac(X, 'act' if sub % 2 else 'dve', lambda o=o, sub=sub: o[:, sub, :], lambda p=p: p[:, :], [p], [o],
                             scale=(scale if isq else 1.0))
                    h0 = (0 if grp < 2 else 8) + (sl % 2) * 4
                    name = 'qT' if isq else 'kT'
                    dst = dr[name][h0:h0 + 4, :, tc * 512:(tc + 1) * 512].rearrange('h p s -> p h s')
                    S.dma('sp', lambda e, dst=dst, o=o: e.dma_start(out=dst, in_=o[:]), reads=[o], writes=[name])
        load_w_slab(X, wf, W[:, 6144:6152], 16, 8)
        for tc in range(cfg.NTC):
            xn = xin[xi % 2]
            xi += 1
            load_xin(X, xn, 'xT', 16, tc * 512, 512)
            p = next_ps(X)
            mm_fm(X, p, wf, 16, 0, 8, xn, 512)
            S.op('dve', lambda e, p=p, tc=tc: e.tensor_copy(fl[:, tc * 512:(tc + 1) * 512], p[0:8, :]), reads=[p], writes=[fl])
        S.dma('sp', lambda e: e.dma_start(out=bfc[:], in_=dr['fox_b_f'][i].rearrange('(h o) -> h o', o=1)), writes=[bfc])
        S.op('dve', lambda e: e.tensor_scalar(bfc[:], bfc[:], -1.0, None, ALU.mult), reads=[bfc], writes=[bfc])
        S.op('pool', lambda e: e.memset(ones8[:], 1.0), writes=[ones8])
        S.op('act', lambda e: e.activation(out=fl[:], in_=fl[:], func=AF.Exp, bias=bfc[:, 0:1], scale=-1.0), reads=[fl, bfc], writes=[fl])
        S.op('act', lambda e: e.activation(out=fl[:], in_=fl[:], func=AF.Ln, bias=1.0, scale=1.0), reads=[fl], writes=[fl])
        S.op('dve', lambda e: e.tensor_tensor_scan(fl[:], ones8[:], fl[:], 0.0, ALU.mult, ALU.add), reads=[fl, ones8], writes=[fl])
        hi = _sb(X, es, 'ep_hi', [8, S_], BF16)
        lo = _sb(X, es, 'ep_lo', [8, S_], BF16)
        nhi = _sb(X, es, 'ep_nhi', [8, S_], BF16)
        nlo = _sb(X, es, 'ep_nlo', [8, S_], BF16)
        one_b = _sb(X, es, 'ep_oneb', [8, S_], BF16)
        hif = _sb(X, es, 'ep_hif', [8, S_], F32)
        S.op('dve', lambda e: e.tensor_copy(hi[:], fl[:]), reads=[fl], writes=[hi])
        S.op('dve', lambda e: e.tensor_copy(hif[:], hi[:]), reads=[hi], writes=[hif])
        S.op('dve', lambda e: e.tensor_tensor(hif[:], fl[:], hif[:], ALU.subtract), reads=[fl, hif], writes=[hif])
        S.op('dve', lambda e: e.tensor_copy(lo[:], hif[:]), reads=[hif], writes=[lo])
        S.op('dve', lambda e: e.tensor_scalar(nhi[:], hi[:], -1.0, None, ALU.mult), reads=[hi], writes=[nhi])
        S.op('dve', lambda e: e.tensor_scalar(nlo[:], lo[:], -1.0, None, ALU.mult), reads=[lo], writes=[nlo])
        S.op('pool', lambda e: e.memset(one_b[:], 1.0), writes=[one_b])
        for r, src in enumerate([hi, lo, one_b, one_b]):
            S.dma('sp', lambda e, r=r, src=src: e.dma_start(out=dr['fxA'][:, r, :], in_=src[:]), reads=[src], writes=['fxA'])
        for r, src in enumerate([one_b, one_b, nhi, nlo]):
            S.dma('sp', lambda e, r=r, src=src: e.dma_start(out=dr['fxB'][:, r, :], in_=src[:]), reads=[src], writes=['fxB'])
        S.flush()


def phase_attention(X, mode):
    S, cfg, dr = X.S, X.cfg, X.dr
    S_ = cfg.S
    NB = S_ // 128
    with ExitStack() as es:
        kT = _sb(X, es, 'at_kT', [128, S_], BF16)
        qT = _sb(X, es, 'at_qT', [128, S_], BF16)
        vt = _sb(X, es, 'at_v', [128, NB, 128], BF16)
        if mode == 'mla':
            kpT = _sb(X, es, 'at_kpT', [64, S_], BF16)
            qpT = _sb(X, es, 'at_qpT', [64, S_], BF16)
            S.dma('sp', lambda e: e.dma_start(out=kpT[:], in_=dr['kpT'][:, :]), reads=['kpT'], writes=[kpT])
        else:
            fa = _sb(X, es, 'at_fa', [4, S_], BF16)
            fb = _sb(X, es, 'at_fb', [4, S_], BF16)
        e1 = [_sb(X, es, 'at_e1%d' % j, [128, 512], F32) for j in range(2)]
        spb = [_sb(X, es, 'at_spb%d' % j, [128, 512], BF16) for j in range(2)]
        tt_ = [_sb(X, es, 'at_t%d' % j, [128, 512], F32) for j in range(2)]
        pT = [_sb(X, es, 'at_pT%d' % j, [128, 512], BF16) for j in range(3)]
        R = _sb(X, es, 'at_R', [128, 512], F32)
        rec = _sb(X, es, 'at_rec', [128, 512], F32)
        ob = [_sb(X, es, 'at_ob%d' % j, [128, 512], BF16) for j in range(2)]
        cnt = 0
        for h in range(16):
            kind = 'mla' if mode == 'mla' else ('sb' if h < 8 else 'fox')
            S.dma('sp', lambda e, h=h: e.dma_start(out=kT[:], in_=dr['kT'][h]), reads=['kT'], writes=[kT])
            S.dma('sp', lambda e, h=h: e.dma_start(out=qT[:], in_=dr['qT'][h]), reads=['qT'], writes=[qT])
            S.dma('sp', lambda e, h=h: e.dma_start(out=vt[:], in_=dr['vtok'][:, h * 128:(h + 1) * 128].rearrange('(b p) d -> p b d', p=128)),
                  reads=['vtok'], writes=[vt])
            if kind == 'mla':
                S.dma('sp', lambda e, h=h: e.dma_start(out=qpT[:], in_=dr['qpT'][h]), reads=['qpT'], writes=[qpT])
            if kind == 'fox':
                S.dma('sp', lambda e, h=h: e.dma_start(out=fa[:], in_=dr['fxA'][h - 8]), reads=['fxA'], writes=[fa])
                S.dma('sp', lambda e, h=h: e.dma_start(out=fb[:], in_=dr['fxB'][h - 8]), reads=['fxB'], writes=[fb])
            for qt in range(S_ // 512):
                qb0 = qt * 4
                q0 = qt * 512
                po = next_ps(X, 0, 2)
                if kind == 'sb':
                    S.op('pool', lambda e: e.memset(R[:], 0.0), writes=[R])
                    S.op('pe', lambda e, po=po, q0=q0: e.matmul(po[:, :], lhsT=X.zeros_b(), rhs=qT[:, q0:q0 + 512], start=True, stop=False),
                         reads=[X.cb, qT], writes=[po])
                    order = list(range(qb0 + 3, -1, -1))
                else:
                    pd = next_ps(X, 2, 4)
                    order = list(range(0, qb0 + 4))
                for ki, kb in enumerate(order):
                    last = (ki == len(order) - 1)
                    first = (ki == 0)
                    i_in = kb - qb0
                    c0 = max(i_in, 0) * 128
                    n = 512 - c0
                    diag = i_in >= 0
                    pz = next_ps(X, 2, 7) if kind == 'sb' else next_ps(X, 4, 7)
                    k0 = kb * 128
                    if kind == 'mla':
                        S.op('pe', lambda e, pz=pz, k0=k0, c0=c0, n=n, q0=q0: e.matmul(pz[:, 0:n], lhsT=kT[:, k0:k0 + 128], rhs=qT[:, q0 + c0:q0 + 512], start=True, stop=False),
                             reads=[kT, qT], writes=[pz])
                        S.op('pe', lambda e, pz=pz, k0=k0, c0=c0, n=n, q0=q0: e.matmul(pz[:, 0:n], lhsT=kpT[:, k0:k0 + 128], rhs=qpT[:, q0 + c0:q0 + 512], start=False, stop=True),
                             reads=[kpT, qpT], writes=[pz])
                    elif kind == 'fox':
                        S.op('pe', lambda e, pz=pz, k0=k0, c0=c0, n=n, q0=q0: e.matmul(pz[:, 0:n], lhsT=kT[:, k0:k0 + 128], rhs=qT[:, q0 + c0:q0 + 512], start=True, stop=False),
                             reads=[kT, qT], writes=[pz])
                        S.op('pe', lambda e, pz=pz, k0=k0, c0=c0, n=n, q0=q0: e.matmul(pz[:, 0:n], lhsT=fa[:, k0:k0 + 128], rhs=fb[:, q0 + c0:q0 + 512], start=False, stop=True),
                             reads=[fa, fb], writes=[pz])
                    else:
                        S.op('pe', lambda e, pz=pz, k0=k0, c0=c0, n=n, q0=q0: e.matmul(pz[:, 0:n], lhsT=kT[:, k0:k0 + 128], rhs=qT[:, q0 + c0:q0 + 512], start=True, stop=False),
                             reads=[kT, qT], writes=[pz])
                    p_t = pT[cnt % 3]
                    if kind == 'sb':
                        e1t = e1[cnt % 2]
                        spt = spb[cnt % 2]
                        t_t = tt_[cnt % 2]
                        pc = next_ps(X, 2, 7)
                        S.op('act', lambda e, pz=pz, n=n, e1t=e1t: e.activation(out=e1t[:, 0:n], in_=pz[:, 0:n], func=AF.Exp), reads=[pz], writes=[e1t])
                        S.op('act', lambda e, n=n, e1t=e1t, spt=spt: e.activation(out=spt[:, 0:n], in_=e1t[:, 0:n], func=AF.Ln, bias=1.0), reads=[e1t], writes=[spt])
                        if diag:
                            S.op('pool', lambda e, spt=spt: e.tensor_tensor(spt[:, 0:128], spt[:, 0:128], X.lt_b(), ALU.mult), reads=[spt, X.cb], writes=[spt])
                        S.op('pe', lambda e, pz=pz, n=n, spt=spt: e.matmul(pz[:, 0:n], lhsT=X.nge_b(), rhs=spt[:, 0:n], start=False, stop=True),
                             reads=[spt, X.cb], writes=[pz])
                        S.op('pe', lambda e, pc=pc, n=n, spt=spt: e.matmul(pc[:, 0:n], lhsT=X.ones_b(), rhs=spt[:, 0:n], start=True, stop=True),
                             reads=[spt, X.cb], writes=[pc])
                        S.op('dve', lambda e, pz=pz, n=n, c0=c0, t_t=t_t: e.tensor_tensor(t_t[:, 0:n], pz[:, 0:n], R[:, c0:512], ALU.subtract),
                             reads=[pz, R], writes=[t_t])
                        S.op('dve', lambda e, pc=pc, n=n, c0=c0: e.tensor_tensor(R[:, c0:512], R[:, c0:512], pc[:, 0:n], ALU.add), reads=[pc, R], writes=[R])
                        S.op('act', lambda e, n=n, t_t=t_t, p_t=p_t: e.activation(out=p_t[:, 0:n], in_=t_t[:, 0:n], func=AF.Exp), reads=[t_t], writes=[p_t])
                        if diag:
                            S.op('pool', lambda e, p_t=p_t: e.tensor_tensor(p_t[:, 0:128], p_t[:, 0:128], X.lt_b(), ALU.mult), reads=[p_t, X.cb], writes=[p_t])
                    else:
                        S.op('act', lambda e, pz=pz, n=n, p_t=p_t: e.activation(out=p_t[:, 0:n], in_=pz[:, 0:n], func=AF.Exp), reads=[pz], writes=[p_t])
                        if diag:
                            S.op('pool', lambda e, p_t=p_t: e.tensor_tensor(p_t[:, 0:128], p_t[:, 0:128], X.le_b(), ALU.mult),
                                 reads=[p_t, X.cb], writes=[p_t])
                        S.op('pe', lambda e, pd=pd, c0=c0, n=n, p_t=p_t, first=first, last=last: e.matmul(pd[:, c0:512], lhsT=X.ones_b(), rhs=p_t[:, 0:n], start=first, stop=last),
                             reads=[p_t, X.cb], writes=[pd])
                    stf = (first and kind != 'sb')
                    S.op('pe', lambda e, po=po, c0=c0, n=n, p_t=p_t, kb=kb, stf=stf, last=last: e.matmul(po[:, c0:512], lhsT=vt[:, kb, :], rhs=p_t[:, 0:n], start=stf, stop=last),
                         reads=[p_t, vt], writes=[po])
                    cnt += 1
                o_t = ob[(h * 8 + qt) % 2]
                if kind == 'sb':
                    S.op('act', lambda e, po=po, o_t=o_t: e.activation(out=o_t[:], in_=po[:, :], func=AF.Copy), reads=[po], writes=[o_t])
                else:
                    S.op('dve', lambda e, pd=pd: e.reciprocal(rec[:], pd[:, :]), reads=[pd], writes=[rec])
                    S.op('dve', lambda e, po=po, o_t=o_t: e.tensor_tensor(o_t[:], po[:, :], rec[:], ALU.mult), reads=[po, rec], writes=[o_t])
                S.dma('sp', lambda e, h=h, q0=q0, o_t=o_t: e.dma_start(out=dr['oT'][h][:, q0:q0 + 512], in_=o_t[:]), reads=[o_t], writes=['oT'])
        S.flush()


def phase_outproj_ln_route(X, layer, wo):
    S, cfg, dr = X.S, X.cfg, X.dr
    D, E, C = cfg.D, cfg.E, cfg.C
    with ExitStack() as es:
        wot = _sb(X, es, 'op_w', [128, 16, D], BF16)
        oTt = [_sb(X, es, 'op_oT%d' % j, [128, 16, 128], BF16) for j in range(2)]
        xr = [_sb(X, es, 'op_xr%d' % j, [128, D], F32) for j in range(2)]
        r = _sb(X, es, 'op_r', [128, D], F32)
        xnew = [_sb(X, es, 'op_xn%d' % j, [128, D], F32) for j in range(2)]
        xb = [_sb(X, es, 'op_xb%d' % j, [128, D], BF16) for j in range(2)]
        xT32 = _sb(X, es, 'op_xT32', [128, 16, 128], F32)
        rw = _sb(X, es, 'op_rw', [128, 16, E], F32)
        rb = _sb(X, es, 'op_rb', [128, E], F32)
        base = _sb(X, es, 'op_base', [128, E], F32)
        T = {'st': _sb(X, es, 'op_st', [128, 4, 6], F32), 'mv': _sb(X, es, 'op_mv', [128, 2], F32),
             'rstd': _sb(X, es, 'op_rstd', [128, 1], F32), 'nmr': _sb(X, es, 'op_nmr', [128, 1], F32)}
        sm = {n: _sb(X, es, 'op_' + n, [128, E], F32) for n in ['lg', 'mask', 'eg', 'gate', 'posf', 'valid', 'k1', 'negkey', 'junk']}
        maskb = _sb(X, es, 'op_maskb', [128, E], BF16)
        t8 = _sb(X, es, 'op_t8', [128, 8], F32)
        t8n = _sb(X, es, 'op_t8n', [128, 8], F32)
        col = {n: _sb(X, es, 'op_c' + n, [128, 1], F32) for n in ['nmx', 'den', 'rden']}
        for c in range(4):
            S.dma('pool', lambda e, c=c: e.dma_start(out=wot[:, :, c * 512:(c + 1) * 512],
                                                         in_=wo[:, c * 512:(c + 1) * 512].rearrange('(k p) n -> p k n', p=128)), writes=[wot])
        load_bcast(X, X.gbc, dr['ln_mix_g'][layer:layer + 1, :], D)
        load_bcast(X, X.bbc, dr['ln_mix_b'][layer:layer + 1, :], D)
        S.dma('sp', lambda e: e.dma_start(out=rw[:], in_=dr['router_w'][layer].rearrange('(k p) n -> p k n', p=128)), writes=[rw])
        load_bcast(X, rb, dr['router_b'][layer:layer + 1, :], E)
        S.op('pool', lambda e: e.memset(base[:], 0.0), writes=[base])
        eCmB = lambda: X.cm[:, 32:32 + E]
        for t in range(cfg.NT):
            ot = oTt[t % 2]
            xrt = xr[t % 2]
            xn = xnew[t % 2]
            xbt = xb[t % 2]
            S.dma('sp', lambda e, t=t, ot=ot: e.dma_start(out=ot[:], in_=dr['oT'][:, :, t * 128:(t + 1) * 128].rearrange('k p s -> p k s')),
                  reads=['oT'], writes=[ot])
            S.dma('sp', lambda e, t=t, xrt=xrt: e.dma_start(out=xrt[:], in_=dr['xres'][t * 128:(t + 1) * 128, :]), reads=['xres'], writes=[xrt])
            for c in range(4):
                p = next_ps(X, 0, 4)
                for kc in range(16):
                    S.op('pe', lambda e, p=p, kc=kc, c=c, ot=ot: e.matmul(p[:, :], lhsT=ot[:, kc, :], rhs=wot[:, kc, c * 512:(c + 1) * 512],
                                                                         start=(kc == 0), stop=(kc == 15)), reads=[ot, wot], writes=[p])
                S.op('dve', lambda e, p=p, c=c, xrt=xrt: e.scalar_tensor_tensor(r[:, c * 512:(c + 1) * 512], xrt[:, c * 512:(c + 1) * 512], cfg.alpha, p[:, :],
                                                                               ALU.mult, ALU.add), reads=[p, xrt], writes=[r])
            layer_norm_tile(X, T, r, xn)
            S.dma('sp', lambda e, t=t, xn=xn: e.dma_start(out=dr['xres'][t * 128:(t + 1) * 128, :], in_=xn[:]), reads=[xn], writes=['xres'])
            for g in range(4):
                p = next_ps(X, 4, 7)
                for j in range(4):
                    kc = g * 4 + j
                    S.op('pe', lambda e, p=p, j=j, kc=kc, xn=xn: e.transpose(p[:, j * 128:(j + 1) * 128], xn[:, kc * 128:(kc + 1) * 128], X.ident_f()),
                         reads=[xn, X.cf], writes=[p])
                evac(X, 'act' if g % 2 else 'dve', lambda g=g: xT32[:, g * 4:(g + 1) * 4, :], lambda p=p: p[:, :].rearrange('p (a b) -> p a b', a=4), [p], [xT32])
            pl = next_ps(X, 0, 4)
            for kc in range(16):
                S.op('pe', lambda e, pl=pl, kc=kc: e.matmul(pl[:, 0:E], lhsT=xT32[:, kc, :], rhs=rw[:, kc, :], start=(kc == 0), stop=(kc == 15)),
                     reads=[xT32, rw], writes=[pl])
            lg, mask, eg, gate, posf, valid, k1, negkey, junk = [sm[n] for n in ['lg', 'mask', 'eg', 'gate', 'posf', 'valid', 'k1', 'negkey', 'junk']]
            S.op('dve', lambda e, pl=pl: e.tensor_tensor(lg[:], pl[:, 0:E], rb[:], ALU.add), reads=[pl, rb], writes=[lg])
            S.op('dve', lambda e: e.max(t8[:], lg[:]), reads=[lg], writes=[t8])
            S.op('dve', lambda e: e.tensor_scalar(mask[:], lg[:], t8[:, 3:4], 0.0, ALU.subtract, ALU.add), reads=[lg, t8], writes=[mask])
            S.op('dve', lambda e: e.tensor_scalar(mask[:], mask[:], 0.0, None, ALU.is_ge), reads=[mask], writes=[mask])
            S.op('dve', lambda e: e.tensor_scalar(col['nmx'][:], t8[:, 0:1], -1.0, None, ALU.mult), reads=[t8], writes=[col['nmx']])
            S.op('act', lambda e: e.activation(out=eg[:], in_=lg[:], func=AF.Exp, bias=col['nmx'][:, 0:1], scale=1.0), reads=[lg, col['nmx']], writes=[eg])
            S.op('dve', lambda e: e.tensor_tensor(eg[:], eg[:], mask[:], ALU.mult), reads=[eg, mask], writes=[eg])
            S.op('dve', lambda e: e.tensor_reduce(col['den'][:], eg[:], AX.X, ALU.add), reads=[eg], writes=[col['den']])
            S.op('dve', lambda e: e.reciprocal(col['rden'][:], col['den'][:]), reads=[col['den']], writes=[col['rden']])
            S.op('dve', lambda e: e.tensor_scalar(gate[:], eg[:], col['rden'][:, 0:1], 1.0, ALU.mult, ALU.mult), reads=[eg, col['rden']], writes=[gate])
            S.op('dve', lambda e: e.tensor_copy(maskb[:], mask[:]), reads=[mask], writes=[maskb])
            pp = next_ps(X, 0, 4)
            S.op('pe', lambda e, pp=pp: e.matmul(pp[:, 0:E], lhsT=X.lt_b(), rhs=maskb[:], start=True, stop=True), reads=[maskb, X.cb], writes=[pp])
            S.op('pe', lambda e, pp=pp: e.matmul(pp[:, 64:64 + E], lhsT=X.ones_b(), rhs=maskb[:], start=True, stop=True), reads=[maskb, X.cb], writes=[pp])
            S.op('dve', lambda e, pp=pp: e.tensor_tensor(posf[:], pp[:, 0:E], base[:], ALU.add), reads=[pp, base], writes=[posf])
            S.op('dve', lambda e, pp=pp: e.tensor_tensor(base[:], pp[:, 64:64 + E], base[:], ALU.add), reads=[pp, base], writes=[base])
            S.op('dve', lambda e: e.tensor_scalar(valid[:], posf[:], float(C), None, ALU.is_lt), reads=[posf], writes=[valid])
            S.op('dve', lambda e: e.tensor_tensor(valid[:], valid[:], mask[:], ALU.mult), reads=[valid, mask], writes=[valid])
            S.op('dve', lambda e: e.tensor_tensor(k1[:], posf[:], eCmB(), ALU.add), reads=[posf, X.cm], writes=[k1])
            S.op('dve', lambda e: e.tensor_tensor(k1[:], k1[:], valid[:], ALU.mult), reads=[k1, valid], writes=[k1])
            S.op('dve', lambda e: e.tensor_scalar(negkey[:], k1[:], -1.0, -cfg.BIG, ALU.mult, ALU.add), reads=[k1], writes=[negkey])
            S.op('dve', lambda e: e.max(t8n[:], negkey[:]), reads=[negkey], writes=[t8n])
            S.op('dve', lambda e, t=t: e.tensor_scalar(X.slots_all[:, t, :], t8n[:, 0:4], -1.0, None, ALU.mult), reads=[t8n], writes=[X.slots_all])
            S.op('dve', lambda e: e.tensor_tensor(gate[:], gate[:], valid[:], ALU.mult), reads=[gate, valid], writes=[gate])
            for k in range(4):
                S.op('dve', lambda e, k=k: e.scalar_tensor_tensor(junk[:], negkey[:], t8n[:, k:k + 1], gate[:], ALU.is_equal, ALU.mult),
                     reads=[negkey, t8n, gate], writes=[junk])
                S.op('dve', lambda e, k=k, t=t: e.tensor_reduce(X.gates_all[:, t, k:k + 1], junk[:], AX.X, ALU.add), reads=[junk], writes=[X.gates_all])
            S.op('act', lambda e, xn=xn, xbt=xbt: e.activation(out=xbt[:], in_=xn[:], func=AF.Copy), reads=[xn], writes=[xbt])
            for k in range(4):
                S.dma('pool', lambda e, k=k, t=t, xbt=xbt: e.indirect_dma_start(
                    out=dr['xslots'][:, :], out_offset=bass.IndirectOffsetOnAxis(ap=X.slots_all[:, t, k:k + 1], axis=0),
                    in_=xbt[:], in_offset=None, bounds_check=bc_reg(X, e), oob_is_err=False),
                    reads=[xbt, X.slots_all], writes=['xslots'])
        S.flush()


def phase_experts(X, layer):
    S, cfg, dr = X.S, X.cfg, X.dr
    D, E, C, F_ = cfg.D, cfg.E, cfg.C, cfg.F
    NJ = C // 128
    chunks = [(n0, min(512, C - n0)) for n0 in range(0, C, 512)]
    with ExitStack() as es:
        wb = [_sb(X, es, 'ex_w%d' % j, [128, 12288], BF16) for j in range(4)]
        xs = [_sb(X, es, 'ex_xs%d' % j, [128, D], BF16) for j in range(2)]
        xsT = _sb(X, es, 'ex_xsT', [128, 16, C], BF16)
        actT = _sb(X, es, 'ex_actT', [128, 6, C], BF16)
        gc = [_sb(X, es, 'ex_gc%d' % j, [128, 512], F32) for j in range(2)]
        sg = [_sb(X, es, 'ex_sg%d' % j, [128, 512], F32) for j in range(2)]
        uc = [_sb(X, es, 'ex_uc%d' % j, [128, 512], F32) for j in range(2)]
        y = [_sb(X, es, 'ex_y%d' % j, [128, D], F32) for j in range(2)]
        bdn = _sb(X, es, 'ex_bdn', [128, D], F32)
        braw = _sb(X, es, 'ex_braw', [128, 128], F32)
        bguT = _sb(X, es, 'ex_bguT', [128, E * 12], F32)
        bsrc = dr['expert_b_gate_up'][layer].rearrange('e (c p) -> (e c) p', p=128)
        nrows = E * 12
        for r0 in range(0, nrows, 128):
            nr = min(128, nrows - r0)
            S.dma('sp', lambda e, r0=r0, nr=nr: e.dma_start(out=braw[0:nr, :], in_=bsrc[r0:r0 + nr, :]), writes=[braw])
            p = next_ps(X, 0, 4)
            S.op('pe', lambda e, p=p, nr=nr: e.transpose(p[:, 0:nr], braw[0:nr, :], X.cf[0:nr, 0:nr]), reads=[braw, X.cf], writes=[p])
            S.op('dve', lambda e, p=p, r0=r0, nr=nr: e.tensor_copy(bguT[:, r0:r0 + nr], p[:, 0:nr]), reads=[p], writes=[bguT])
        cnt = 0
        gs = _sb(X, es, 'ex_gs', [128, 6, C], BF16)

        def wview(n, k):
            b = wb[n % 4]
            return b, (lambda b=b, k=k: b[:].rearrange('p (k n) -> p k n', k=k))

        def issue_load(ex, which):
            if ex >= E:
                return
            Wgu = dr['expert_w_gate_up'][layer, ex]
            Wdn = dr['expert_w_down'][layer, ex]
            if which == 0:
                bg_, gv = wview(3 * ex, 16)
                S.dma('pool', lambda e, gv=gv, Wgu=Wgu: e.dma_start(out=gv(), in_=Wgu[:, 0:F_].rearrange('(k p) n -> p k n', p=128)), writes=[bg_])
            elif which == 1:
                bu_, uv = wview(3 * ex + 1, 16)
                S.dma('pool', lambda e, uv=uv, Wgu=Wgu: e.dma_start(out=uv(), in_=Wgu[:, F_:2 * F_].rearrange('(k p) n -> p k n', p=128)), writes=[bu_])
            else:
                bd_, dv = wview(3 * ex + 2, 6)
                S.dma('pool', lambda e, dv=dv, Wdn=Wdn: e.dma_start(out=dv(), in_=Wdn.rearrange('(k p) n -> p k n', p=128)), writes=[bd_])

        issue_load(0, 0)
        issue_load(0, 1)
        issue_load(0, 2)
        for ex in range(E):
            wg, wgv = wview(3 * ex, 16)
            wu, wuv = wview(3 * ex + 1, 16)
            wd, wdv = wview(3 * ex + 2, 6)
            issue_load(ex + 1, 0)
            load_bcast(X, bdn, dr['expert_b_down'][layer, ex:ex + 1, :], D)
            for j in range(NJ):
                xt = xs[j % 2]
                r0 = ex * C + j * 128
                S.dma('sp', lambda e, xt=xt, r0=r0: e.dma_start(out=xt[:], in_=dr['xslots'][r0:r0 + 128, :]), reads=['xslots'], writes=[xt])
                for g in range(2):
                    p = X.pb
                    pv = lambda: X.pb[:]
                    for jj in range(8):
                        kc = g * 8 + jj
                        S.op('pe', lambda e, pv=pv, jj=jj, kc=kc, xt=xt: e.transpose(pv()[:, jj * 128:(jj + 1) * 128], xt[:, kc * 128:(kc + 1) * 128], X.ident_b()),
                             reads=[xt, X.cb], writes=[p])
                    evac(X, 'act' if g else 'dve', lambda g=g, j=j: xsT[:, g * 8:(g + 1) * 8, j * 128:(j + 1) * 128],
                         lambda pv=pv: pv().rearrange('p (a b) -> p a b', a=8), [p], [xsT])
            for fc in range(6):
                for (n0, n) in chunks:
                    pg = next_ps(X, 4, 7)
                    for kc in range(16):
                        S.op('pe', lambda e, pg=pg, kc=kc, fc=fc, n0=n0, n=n, wgv=wgv: e.matmul(pg[:, 0:n], lhsT=wgv()[:, kc, fc * 128:(fc + 1) * 128],
                                                                                                rhs=xsT[:, kc, n0:n0 + n], start=(kc == 0), stop=(kc == 15)),
                             reads=[wg, xsT], writes=[pg])
                    g_t, s_t = gc[cnt % 2], sg[cnt % 2]
                    cnt += 1
                    bg = lambda ex=ex, fc=fc: bguT[:, ex * 12 + fc:ex * 12 + fc + 1]
                    S.op('dve', lambda e, pg=pg, n=n, g_t=g_t, bg=bg: e.tensor_scalar(g_t[:, 0:n], pg[:, 0:n], bg(), 7.0, ALU.add, ALU.min), reads=[pg, bguT], writes=[g_t])
                    S.op('act', lambda e, n=n, g_t=g_t, s_t=s_t: e.activation(out=s_t[:, 0:n], in_=g_t[:, 0:n], func=AF.Sigmoid, scale=1.702), reads=[g_t], writes=[s_t])
                    S.op('dve', lambda e, n=n, n0=n0, fc=fc, g_t=g_t, s_t=s_t: e.tensor_tensor(gs[:, fc, n0:n0 + n], g_t[:, 0:n], s_t[:, 0:n], ALU.mult), reads=[g_t, s_t], writes=[gs])
            issue_load(ex + 1, 1)
            for fc in range(6):
                for (n0, n) in chunks:
                    pu = next_ps(X, 4, 7)
                    for kc in range(16):
                        S.op('pe', lambda e, pu=pu, kc=kc, fc=fc, n0=n0, n=n, wuv=wuv: e.matmul(pu[:, 0:n], lhsT=wuv()[:, kc, fc * 128:(fc + 1) * 128],
                                                                                                rhs=xsT[:, kc, n0:n0 + n], start=(kc == 0), stop=(kc == 15)),
                             reads=[wu, xsT], writes=[pu])
                    u_t = uc[cnt % 2]
                    cnt += 1
                    bu = lambda ex=ex, fc=fc: bguT[:, ex * 12 + 6 + fc:ex * 12 + 6 + fc + 1]
                    S.op('dve', lambda e, pu=pu, n=n, u_t=u_t, bu=bu: e.tensor_scalar(u_t[:, 0:n], pu[:, 0:n], bu(), 7.0, ALU.add, ALU.min), reads=[pu, bguT], writes=[u_t])
                    S.op('dve', lambda e, n=n, u_t=u_t: e.tensor_scalar(u_t[:, 0:n], u_t[:, 0:n], -7.0, 1.0, ALU.max, ALU.add), reads=[u_t], writes=[u_t])
                    S.op('dve', lambda e, n=n, n0=n0, fc=fc, u_t=u_t: e.tensor_tensor(actT[:, fc, n0:n0 + n], gs[:, fc, n0:n0 + n], u_t[:, 0:n], ALU.mult),
                         reads=[gs, u_t], writes=[actT])
            issue_load(ex + 1, 2)
            for j in range(NJ):
                yt = y[j % 2]
                for c in range(4):
                    p = next_ps(X, 0, 4)
                    for fc in range(6):
                        S.op('pe', lambda e, p=p, fc=fc, j=j, c=c, wdv=wdv: e.matmul(p[:, :], lhsT=actT[:, fc, j * 128:(j + 1) * 128], rhs=wdv()[:, fc, c * 512:(c + 1) * 512],
                                                                                     start=(fc == 0), stop=(fc == 5)), reads=[actT, wd], writes=[p])
                    S.op('dve', lambda e, p=p, c=c, yt=yt: e.tensor_tensor(yt[:, c * 512:(c + 1) * 512], p[:, :], bdn[:, c * 512:(c + 1) * 512], ALU.add),
                         reads=[p, bdn], writes=[yt])
                r0 = ex * C + j * 128
                S.dma('sp', lambda e, yt=yt, r0=r0: e.dma_start(out=dr['yslots'][r0:r0 + 128, :], in_=yt[:]), reads=[yt], writes=['yslots'])
        S.flush()


def phase_combine_ln(X, layer, last):
    S, cfg, dr = X.S, X.cfg, X.dr
    D = cfg.D
    with ExitStack() as es:
        yb = [_sb(X, es, 'cb_y%d' % j, [128, D], F32) for j in range(8)]
        xr = [_sb(X, es, 'cb_xr%d' % j, [128, D], F32) for j in range(2)]
        r = _sb(X, es, 'cb_r', [128, D], F32)
        xnew = [_sb(X, es, 'cb_xn%d' % j, [128, D], F32) for j in range(2)]
        xb = [_sb(X, es, 'cb_xb%d' % j, [128, D], BF16) for j in range(2)]
        xTt = [_sb(X, es, 'cb_xT%d' % j, [128, 16, 128], BF16) for j in range(2)]
        T = {'st': _sb(X, es, 'cb_st', [128, 4, 6], F32), 'mv': _sb(X, es, 'cb_mv', [128, 2], F32),
             'rstd': _sb(X, es, 'cb_rstd', [128, 1], F32), 'nmr': _sb(X, es, 'cb_nmr', [128, 1], F32)}
        load_bcast(X, X.gbc, dr['ln_ffn_g'][layer:layer + 1, :], D)
        load_bcast(X, X.bbc, dr['ln_ffn_b'][layer:layer + 1, :], D)
        for k in range(8):
            S.op('pool', lambda e, k=k: e.memset(yb[k][:], 0.0), writes=[yb[k]])
        for t in range(cfg.NT):
            xrt = xr[t % 2]
            xn = xnew[t % 2]
            S.dma('sp', lambda e, t=t, xrt=xrt: e.dma_start(out=xrt[:], in_=dr['xres'][t * 128:(t + 1) * 128, :]), reads=['xres'], writes=[xrt])
            ybt = yb[(t % 2) * 4:(t % 2) * 4 + 4]
            for k in range(4):
                S.dma('pool', lambda e, k=k, t=t, ybt=ybt: e.indirect_dma_start(
                    out=ybt[k][:], out_offset=None, in_=dr['yslots'][:, :],
                    in_offset=bass.IndirectOffsetOnAxis(ap=X.slots_all[:, t, k:k + 1], axis=0),
                    bounds_check=bc_reg(X, e), oob_is_err=False), reads=['yslots', X.slots_all], writes=[ybt[k]])
            S.op('act', lambda e, xrt=xrt: e.activation(out=r[:], in_=xrt[:], func=AF.Copy, scale=cfg.alpha), reads=[xrt], writes=[r])
            for k in range(4):
                S.op('dve', lambda e, k=k, t=t, ybt=ybt: e.scalar_tensor_tensor(r[:], ybt[k][:], X.gates_all[:, t, k:k + 1], r[:], ALU.mult, ALU.add),
                     reads=[ybt[k], X.gates_all, r], writes=[r])
            layer_norm_tile(X, T, r, xn, mult_eng='dve')
            if last:
                S.dma('sp', lambda e, t=t, xn=xn: e.dma_start(out=dr['out'][t * 128:(t + 1) * 128, :], in_=xn[:]), reads=[xn], writes=['out'])
            else:
                S.dma('sp', lambda e, t=t, xn=xn: e.dma_start(out=dr['xres'][t * 128:(t + 1) * 128, :], in_=xn[:]), reads=[xn], writes=['xres'])
                make_xT_tile(X, {'xb': xb[t % 2], 'xTt': xTt[t % 2]}, xn, t)
        S.flush()


def phase_mla_proj(X, i):
    S, cfg, dr = X.S, X.cfg, X.dr
    S_ = cfg.S
    nc = X.nc
    scale = 192 ** -0.5
    if 'cqT' not in dr:
        dr['cqT'] = nc.dram_tensor('cqT', [4, 128, S_], BF16).ap()
        dr['ckvT'] = nc.dram_tensor('ckvT', [4, 128, S_], BF16).ap()
        dr['cosT'] = nc.dram_tensor('cosT', [64, S_], F32).ap()
        dr['sinT'] = nc.dram_tensor('sinT', [64, S_], F32).ap()
    Wd = dr['mla_w_down'][i]
    TWO_PI = 6.283185307179586
    with ExitStack() as es:
        wdn = _sb(X, es, 'm1_wdn', [128, 16, 1088], BF16)
        wrot = _sb(X, es, 'm1_wrot', [128, 16, 64], BF16)
        xin = [_sb(X, es, 'm1_x%d' % j, [128, 16, 512], BF16) for j in range(2)]
        posi = _sb(X, es, 'm1_posi', [64, S_], I32)
        ang = _sb(X, es, 'm1_ang', [64, S_], F32)
        cs = _sb(X, es, 'm1_cos', [64, S_], F32)
        sn = _sb(X, es, 'm1_sin', [64, S_], F32)
        raw = _sb(X, es, 'm1_raw', [128, 4, 512], F32)
        sq = [_sb(X, es, 'm1_sq%d' % j, [128, 512], BF16) for j in range(2)]
        sqr = _sb(X, es, 'm1_sqr', [128, 512], F32)
        rstd = _sb(X, es, 'm1_rstd', [128, 512], F32)
        cn = [_sb(X, es, 'm1_cn%d' % j, [128, 4, 512], BF16) for j in range(2)]
        nrm = _sb(X, es, 'm1_nrm', [128, 8], F32)
        t1 = _sb(X, es, 'm1_t1', [64, 512], F32)
        t2 = _sb(X, es, 'm1_t2', [64, 512], F32)
        kpo = [_sb(X, es, 'm1_kpo%d' % j, [64, 512], BF16) for j in range(2)]
        for c0 in range(0, 1088, 544):
            S.dma('pool', lambda e, c0=c0: e.dma_start(out=wdn[:, :, c0:c0 + 544], in_=Wd[:, c0:c0 + 544].rearrange('(k p) n -> p k n', p=128)), writes=[wdn])
        S.op('act', lambda e: e.activation(out=wrot[:, :, 0:32], in_=wdn[:, :, 1056:1088], func=AF.Copy, scale=-1.0), reads=[wdn], writes=[wrot])
        S.op('dve', lambda e: e.tensor_copy(wrot[:, :, 32:64], wdn[:, :, 1024:1056]), reads=[wdn], writes=[wrot])
        S.dma('sp', lambda e: e.dma_start(out=nrm[:, 0:4], in_=dr['mla_q_norm'][i].rearrange('(c p) -> p c', p=128), allow_slow_non_contiguous=True), writes=[nrm])
        S.dma('sp', lambda e: e.dma_start(out=nrm[:, 4:8], in_=dr['mla_kv_norm'][i].rearrange('(c p) -> p c', p=128), allow_slow_non_contiguous=True), writes=[nrm])
        LV = int(os.environ.get('KDBG_MLA', '9'))
        S.dma('sp', lambda e: e.dma_start(out=posi[:], in_=dr['positions'][0:1, :].partition_broadcast(64)), writes=[posi])
        S.op('dve', lambda e: e.tensor_copy(ang[:], posi[:]), reads=[posi], writes=[ang])
        S.op('dve', lambda e: e.tensor_scalar(ang[:], ang[:], X.cm[0:64, 0:1], 1.0, ALU.mult, ALU.mult), reads=[ang, X.cm], writes=[ang])
        kf = _sb(X, es, 'm1_kf', [64, S_], F32)
        C1 = 6.28125
        C2 = TWO_PI - C1
        PI_LO = 3.1415925

        def sin_of(dst, add):
            S.op('dve', lambda e: e.tensor_scalar(dst[:], ang[:], add, None, ALU.add), reads=[ang], writes=[dst])
            S.op('dve', lambda e: e.tensor_scalar(kf[:], dst[:], 1.0 / TWO_PI, None, ALU.mult), reads=[dst], writes=[kf])
            S.op('dve', lambda e: e.tensor_copy(posi[:], kf[:]), reads=[kf], writes=[posi])
            S.op('dve', lambda e: e.tensor_copy(kf[:], posi[:]), reads=[posi], writes=[kf])
            S.op('dve', lambda e: e.scalar_tensor_tensor(dst[:], kf[:], -C1, dst[:], ALU.mult, ALU.add), reads=[kf, dst], writes=[dst])
            S.op('dve', lambda e: e.scalar_tensor_tensor(dst[:], kf[:], -C2, dst[:], ALU.mult, ALU.add), reads=[kf, dst], writes=[dst])
            S.op('dve', lambda e: e.tensor_scalar(kf[:], dst[:], 3.141592653589793, None, ALU.is_gt), reads=[dst], writes=[kf])
            S.op('dve', lambda e: e.scalar_tensor_tensor(dst[:], kf[:], -TWO_PI, dst[:], ALU.mult, ALU.add), reads=[kf, dst], writes=[dst])
            S.op('dve', lambda e: e.tensor_scalar(kf[:], dst[:], -3.141592653589793, None, ALU.is_lt), reads=[dst], writes=[kf])
            S.op('dve', lambda e: e.scalar_tensor_tensor(dst[:], kf[:], TWO_PI, dst[:], ALU.mult, ALU.add), reads=[kf, dst], writes=[dst])
            S.op('dve', lambda e: e.tensor_scalar(dst[:], dst[:], PI_LO, -PI_LO, ALU.min, ALU.max), reads=[dst], writes=[dst])
            S.op('act', lambda e: e.activation(out=dst[:], in_=dst[:], func=AF.Sin), reads=[dst], writes=[dst])
        if LV >= 2:
            sin_of(sn, 0.0)
            sin_of(cs, 1.5707963267948966)
        S.dma('sp', lambda e: e.dma_start(out=dr['cosT'][:, :], in_=cs[:]), reads=[cs], writes=['cosT'])
        S.dma('sp', lambda e: e.dma_start(out=dr['sinT'][:, :], in_=sn[:]), reads=[sn], writes=['sinT'])
        for tc in range(cfg.NTC if LV >= 3 else 0):
            xn = xin[tc % 2]
            load_xin(X, xn, 'xT', 16, tc * 512, 512)
            for which in range(2):
                cnt = cn[which]
                pss = next_ps(X, 5, 7)
                for c in range(4):
                    p = next_ps(X, 0, 5)
                    mm_fm(X, p, wdn, 16, which * 512 + c * 128, 128, xn, 512)
                    S.op('dve', lambda e, p=p, c=c: e.tensor_copy(raw[:, c, :], p[:, :]), reads=[p], writes=[raw])
                    sqt = sq[c % 2]
                    S.op('act', lambda e, c=c, sqt=sqt: e.activation(out=sqt[:], in_=raw[:, c, :], func=AF.Square), reads=[raw], writes=[sqt])
                    S.op('pe', lambda e, pss=pss, sqt=sqt, c=c: e.matmul(pss[:, :], lhsT=X.ones_b(), rhs=sqt[:], start=(c == 0), stop=(c == 3)),
                         reads=[sqt, X.cb], writes=[pss])
                S.op('act', lambda e, pss=pss: e.activation(out=sqr[:], in_=pss[:, :], func=AF.Sqrt, bias=X.cm[:, 2:3], scale=1.0 / 512), reads=[pss, X.cm], writes=[sqr])
                S.op('dve', lambda e: e.reciprocal(rstd[:], sqr[:]), reads=[sqr], writes=[rstd])
                for c in range(4):
                    S.op('dve', lambda e, c=c, which=which, cnt=cnt: e.scalar_tensor_tensor(cnt[:, c, :], raw[:, c, :], nrm[:, which * 4 + c:which * 4 + c + 1], rstd[:],
                                                                                            ALU.mult, ALU.mult), reads=[raw, nrm, rstd], writes=[cnt])
                name = 'cqT' if which == 0 else 'ckvT'
                dst = dr[name][:, :, tc * 512:(tc + 1) * 512].rearrange('k p s -> p k s')
                S.dma('sp', lambda e, dst=dst, cnt=cnt: e.dma_start(out=dst, in_=cnt[:]), reads=[cnt], writes=[name])
            if os.environ.get('KDBG_NOKPE'):
                continue
            pa = next_ps(X, 0, 5)
            pb = next_ps(X, 0, 5)
            mm_fm(X, pa, wdn, 16, 1024, 64, xn, 512)
            mm_fm(X, pb, wrot, 16, 0, 64, xn, 512)
            ko = kpo[tc % 2]
            S.op('dve', lambda e, pa=pa, tc=tc: e.tensor_tensor(t1[:], pa[0:64, :], cs[:, tc * 512:(tc + 1) * 512], ALU.mult), reads=[pa, cs], writes=[t1])
            S.op('dve', lambda e, pb=pb, tc=tc: e.tensor_tensor(t2[:], pb[0:64, :], sn[:, tc * 512:(tc + 1) * 512], ALU.mult), reads=[pb, sn], writes=[t2])
            S.op('pool', lambda e, ko=ko: e.tensor_tensor(ko[:], t1[:], t2[:], ALU.add), reads=[t1, t2], writes=[ko])
            S.dma('sp', lambda e, ko=ko, tc=tc: e.dma_start(out=dr['kpT'][:, tc * 512:(tc + 1) * 512], in_=ko[:]), reads=[ko], writes=['kpT'])
        S.flush()
    if LV < 4:
        return
    with ExitStack() as es:
        wuq = _sb(X, es, 'm2_wuq', [128, 4, 3072], BF16)
        wqr = _sb(X, es, 'm2_wqr', [128, 4, 1024], BF16)
        wkv = _sb(X, es, 'm2_wkv', [128, 4, 4096], BF16)
        cs = _sb(X, es, 'm2_cos', [64, S_], F32)
        sn = _sb(X, es, 'm2_sin', [64, S_], F32)
        cq = [_sb(X, es, 'm2_cq%d' % j, [128, 4, 512], BF16) for j in range(2)]
        ckv = [_sb(X, es, 'm2_ckv%d' % j, [128, 4, 512], BF16) for j in range(2)]
        ob = [_sb(X, es, 'm2_ob%d' % j, [128, 512], BF16) for j in range(4)]
        vb = [_sb(X, es, 'm2_vb%d' % j, [128, 512], BF16) for j in range(2)]
        t1 = _sb(X, es, 'm2_t1', [64, 512], F32)
        t2 = _sb(X, es, 'm2_t2', [64, 512], F32)
        for c0 in range(0, 3072, 512):
            S.dma('pool', lambda e, c0=c0: e.dma_start(out=wuq[:, :, c0:c0 + 512], in_=dr['mla_w_uq'][i][:, c0:c0 + 512].rearrange('(k p) n -> p k n', p=128)), writes=[wuq])
        for c0 in range(0, 4096, 512):
            S.dma('pool', lambda e, c0=c0: e.dma_start(out=wkv[:, :, c0:c0 + 512], in_=dr['mla_w_ukv'][i][:, c0:c0 + 512].rearrange('(k p) n -> p k n', p=128)), writes=[wkv])
        uqv = lambda: wuq[:].rearrange('p k (h d) -> p k h d', d=192)
        qrv = lambda: wqr[:].rearrange('p k (h d) -> p k h d', d=64)
        for kc in range(4):
            S.op('act', lambda e, kc=kc: e.activation(out=qrv()[:, kc, :, 0:32], in_=uqv()[:, kc, :, 160:192], func=AF.Copy, scale=-1.0), reads=[wuq], writes=[wqr])
            S.op('dve', lambda e, kc=kc: e.tensor_copy(qrv()[:, kc, :, 32:64], uqv()[:, kc, :, 128:160]), reads=[wuq], writes=[wqr])
        S.dma('sp', lambda e: e.dma_start(out=cs[:], in_=dr['cosT'][:, :]), reads=['cosT'], writes=[cs])
        S.dma('sp', lambda e: e.dma_start(out=sn[:], in_=dr['sinT'][:, :]), reads=['sinT'], writes=[sn])
        oi = 0
        for tc in range(cfg.NTC if LV >= 5 else 0):
            cqt = cq[tc % 2]
            ckt = ckv[tc % 2]
            load_xin(X, cqt, 'cqT', 4, tc * 512, 512)
            load_xin(X, ckt, 'ckvT', 4, tc * 512, 512)
            tsl = slice(tc * 512, (tc + 1) * 512)
            for h in range(16):
                p = next_ps(X)
                mm_fm(X, p, wuq, 4, h * 192, 128, cqt, 512)
                o = ob[oi % 4]
                oi += 1
                evac(X, 'act', lambda o=o: o[:], lambda p=p: p[:, :], [p], [o], scale=scale)
                S.dma('sp', lambda e, o=o, h=h, tsl=tsl: e.dma_start(out=dr['qT'][h][:, tsl], in_=o[:]), reads=[o], writes=['qT'])
                p = next_ps(X)
                mm_fm(X, p, wkv, 4, h * 256, 128, ckt, 512)
                o = ob[oi % 4]
                oi += 1
                evac(X, 'dve', lambda o=o: o[:], lambda p=p: p[:, :], [p], [o])
                S.dma('sp', lambda e, o=o, h=h, tsl=tsl: e.dma_start(out=dr['kT'][h][:, tsl], in_=o[:]), reads=[o], writes=['kT'])
                pa = next_ps(X)
                pb = next_ps(X)
                mm_fm(X, pa, wuq, 4, h * 192 + 128, 64, cqt, 512)
                mm_fm(X, pb, wqr, 4, h * 64, 64, cqt, 512)
                o = ob[oi % 4]
                oi += 1
                S.op('dve', lambda e, pa=pa, tsl=tsl: e.tensor_tensor(t1[:], pa[0:64, :], cs[:, tsl], ALU.mult), reads=[pa, cs], writes=[t1])
                S.op('dve', lambda e, pb=pb, tsl=tsl: e.tensor_tensor(t2[:], pb[0:64, :], sn[:, tsl], ALU.mult), reads=[pb, sn], writes=[t2])
                S.op('pool', lambda e: e.tensor_tensor(t1[:], t1[:], t2[:], ALU.add), reads=[t1, t2], writes=[t1])
                S.op('act', lambda e, o=o: e.activation(out=o[0:64, :], in_=t1[:], func=AF.Copy, scale=scale), reads=[t1], writes=[o])
                S.dma('sp', lambda e, o=o, h=h, tsl=tsl: e.dma_start(out=dr['qpT'][h][:, tsl], in_=o[0:64, :]), reads=[o], writes=['qpT'])
            kvv = lambda: wkv[:].rearrange('p k (h two d) -> p k h two d', two=2, d=128)
            for tt in range(4):
                for hg in range(4):
                    p = next_ps(X)
                    for kc in range(4):
                        S.op('pe', lambda e, p=p, kc=kc, tt=tt, hg=hg, ckt=ckt: e.matmul(p[:, :].rearrange('p (h d) -> p h d', d=128), lhsT=ckt[:, kc, tt * 128:(tt + 1) * 128],
                                                                                         rhs=kvv()[:, kc, hg * 4:(hg + 1) * 4, 1, :], start=(kc == 0), stop=(kc == 3)),
                             reads=[ckt, wkv], writes=[p])
                    v_t = vb[(tt * 4 + hg) % 2]
                    evac(X, 'act' if hg % 2 else 'dve', lambda v_t=v_t: v_t[:], lambda p=p: p[:, :], [p], [v_t])
                    r0 = tc * 512 + tt * 128
                    S.dma('sp', lambda e, v_t=v_t, r0=r0, hg=hg: e.dma_start(out=dr['vtok'][r0:r0 + 128, hg * 512:(hg + 1) * 512], in_=v_t[:]), reads=[v_t], writes=['vtok'])
        S.flush()


def make_consts(cfg):
    j = np.arange(128)[:, None]
    t = np.arange(128)[None, :]
    ident = (j == t).astype(np.float32)
    lt = (j < t).astype(np.float32)
    le = (j <= t).astype(np.float32)
    c_f32 = np.zeros((128, 512), np.float32)
    c_f32[:, 0:128] = ident
    c_f32[:, 128:256] = lt
    c_f32[:, 256:384] = le
    c_bf = np.zeros((128, 768), np.float32)
    c_bf[:, 0:128] = ident
    c_bf[:, 128:256] = lt
    c_bf[:, 256:384] = 1.0
    c_bf[:, 384:512] = -(j >= t).astype(np.float32)
    c_bf[:, 640:768] = le
    c_misc = np.zeros((128, 64), np.float32)
    inv_freq = (10000.0 ** (-np.arange(0, 64, 2, dtype=np.float32) / np.float32(64))).astype(np.float32)
    c_misc[0:32, 0] = inv_freq
    c_misc[32:64, 0] = inv_freq
    c_misc[:, 1] = 3.141592
    c_misc[:, 2] = 1e-6
    c_misc[:, 3] = 1e-5
    c_misc[:, 32:32 + cfg.E] = (np.arange(cfg.E, dtype=np.float32) * cfg.C - cfg.BIG)[None, :]
    return {'c_f32': c_f32, 'c_bf': c_bf.astype(ml_dtypes.bfloat16), 'c_misc': c_misc}


_NC_CACHE = {}


def run_cores(cfg, inputs, n_cores):
    key = (cfg.S, cfg.NL, cfg.C, cfg.E, cfg.stop)
    if key not in _NC_CACHE:
        _NC_CACHE[key] = build(cfg)
    nc = _NC_CACHE[key]
    consts = make_consts(cfg)
    in_maps = []
    NE = (cfg.NL + 1) // 2
    NO = max(cfg.NL // 2, 1)
    for c in range(n_cores):
        m = {'x': np.ascontiguousarray(inputs['x'][c], dtype=np.float32),
             'positions': np.ascontiguousarray(inputs['positions'][c:c + 1], dtype=np.int32)}
        for n in ['ln_mix_g', 'ln_mix_b', 'ln_ffn_g', 'ln_ffn_b', 'router_w', 'router_b', 'expert_w_gate_up', 'expert_b_gate_up',
                  'expert_w_down', 'expert_b_down']:
            m[n] = np.asarray(inputs[n][:cfg.NL], dtype=np.float32)
        for n in ['even_w_in', 'fox_b_f', 'even_w_o']:
            m[n] = np.asarray(inputs[n][:NE], dtype=np.float32)
        for n in ['mla_w_down', 'mla_q_norm', 'mla_kv_norm', 'mla_w_uq', 'mla_w_ukv', 'mla_w_o']:
            m[n] = np.asarray(inputs[n][:NO], dtype=np.float32)
        m.update(consts)
        in_maps.append(m)
    res = run_bass_kernel_spmd(nc, in_maps, core_ids=list(range(n_cores)))
    return np.stack([r['out'] for r in res.results], axis=0)


def kernel(**inputs):
    cfg = Cfg(S=4096, NL=4, C=640, E=32)
    out = run_cores(cfg, inputs, 4)
    return out.astype(np.float32)
```
